# Optimizing a Trainium2 kernel written in Bass

```python
import math
import jax, jax.numpy as jnp
from jax import lax
import numpy as np

D_MODEL = 2048
BATCH = 2
SEQ = 8192
DEPTH = 2
DEC_BATCH = 2
DEC_SEQ = 4096
PAST_LEN = 128

GRID_W = 64
HEAD_DIM = 128
N_HEADS_A = 4
B_PAIRS = ((128, 1), (512, 4), (2048, 16))
N_HEADS_B_GROUP = 4
N_HEADS_B = N_HEADS_B_GROUP * len(B_PAIRS)
N_HEADS_QKV = N_HEADS_A + N_HEADS_B
WIN_H = 8
WIN_W = 16
ROPE_THETA = 10000.0
D_FF = 5632
N_EXPERTS = 8
TOP_K = 2
D_FF_EXPERT = 7168
MOE_BLOCK = 128
D_INNER = 2 * D_MODEL
SSM_HEAD_DIM = 64
SSM_HEADS = D_INNER // SSM_HEAD_DIM
SSM_GROUPS = 8
SSM_HPG = SSM_HEADS // SSM_GROUPS
SSM_STATE = 128
SSM_CONV = 5
SSM_CHUNK = 128
CONV_DIM = D_INNER + 2 * SSM_GROUPS * SSM_STATE
IN_PROJ_C = D_INNER + CONV_DIM + 2 * SSM_HEADS
N_EVEN = (DEPTH + 1) // 2
N_ODD = DEPTH // 2
EPS = 1e-6
F32 = jnp.float32

kernel_name = "hybrid_natten_dilated_ssd_moe_encoder"


def rmsnorm(x, g):
    xf = x.astype(F32)
    y = xf * lax.rsqrt(jnp.mean(xf * xf, axis=-1, keepdims=True) + EPS)
    return (y * g.astype(F32)).astype(x.dtype)


def rope(t, pos):
    half = HEAD_DIM // 2
    inv = ROPE_THETA ** (-jnp.arange(half, dtype=F32) / half)
    ang = pos[:, None] * inv[None, :]
    cos = jnp.cos(ang)[None, :, None, :]
    sin = jnp.sin(ang)[None, :, None, :]
    tf = t.astype(F32)
    t1, t2 = tf[..., :half], tf[..., half:]
    return jnp.concatenate([t1 * cos - t2 * sin, t2 * cos + t1 * sin], axis=-1).astype(t.dtype)


def neighbourhood_attention(q, k, v, rpb):
    b, T, h, hd = q.shape
    rows = T // GRID_W
    kh = min(WIN_H, rows)
    qg = q.reshape(b, rows, GRID_W, h, hd)
    kg = k.reshape(b, rows, GRID_W, h, hd)
    vg = v.reshape(b, rows, GRID_W, h, hd)
    r = jnp.arange(rows)
    row_start = jnp.clip(r - WIN_H // 2, 0, rows - kh)
    row_idx = row_start[:, None] + jnp.arange(kh)[None, :]
    k_rb = kg[:, row_idx]
    v_rb = vg[:, row_idx]
    c = jnp.arange(GRID_W)
    col_start = jnp.clip(c - WIN_W // 2, 0, GRID_W - WIN_W)
    col_ok = (c[None, :] >= col_start[:, None]) & (c[None, :] < col_start[:, None] + WIN_W)
    d_row = row_idx - r[:, None] + (WIN_H - 1)
    d_col = jnp.clip(c[None, :] - c[:, None] + (WIN_W - 1), 0, 2 * WIN_W - 2)
    bias = rpb[:, d_row[:, None, :, None], d_col[None, :, None, :]].astype(F32)
    s = jnp.einsum('brqhd,brikhd->bhrqik', qg, k_rb, preferred_element_type=F32) * (hd ** -0.5)
    s = s + bias[None]
    s = jnp.where(col_ok[None, None, None, :, None, :], s, -jnp.inf)
    p = jax.nn.softmax(s.reshape(b, h, rows, GRID_W, kh * GRID_W), axis=-1)
    p = p.reshape(b, h, rows, GRID_W, kh, GRID_W).astype(v.dtype)
    o = jnp.einsum('bhrqik,brikhd->brqhd', p, v_rb)
    return o.reshape(b, T, h, hd)


def dilated_group(q, k, v, window, dilation):
    b, T, h, hd = q.shape
    rad = window // (2 * dilation)
    blk = rad
    n = T // dilation
    nb = -(-n // blk)
    npad = nb * blk

    def sub(t):
        return t.reshape(b, n, dilation, h, hd).transpose(0, 2, 1, 3, 4)

    def kwin(t):
        tp = jnp.pad(sub(t), ((0, 0), (0, 0), (blk, npad - n + blk), (0, 0), (0, 0)))
        tp = tp.reshape(b, dilation, nb + 2, blk, h, hd)
        return jnp.concatenate([tp[:, :, :-2], tp[:, :, 1:-1], tp[:, :, 2:]], axis=3)

    qs = jnp.pad(sub(q), ((0, 0), (0, 0), (0, npad - n), (0, 0), (0, 0))).reshape(b, dilation, nb, blk, h, hd)
    kw = kwin(k)
    vw = kwin(v)
    s = jnp.einsum('bcjqhe,bcjkhe->bchjqk', qs, kw, preferred_element_type=F32) * (hd ** -0.5)
    qm = jnp.arange(nb)[:, None, None] * blk + jnp.arange(blk)[None, :, None]
    km = (jnp.arange(nb)[:, None, None] - 1) * blk + jnp.arange(3 * blk)[None, None, :]
    ok = ((jnp.abs(km - qm) <= rad) & (km >= 0) & (km < n)) | (qm >= n)
    s = jnp.where(ok, s, -jnp.inf)
    m = jnp.max(s, axis=-1, keepdims=True)
    e = jnp.exp(s - m)
    den = jnp.sum(e, axis=-1)
    o = jnp.einsum('bchjqk,bcjkhe->bcjqhe', e.astype(v.dtype), vw, preferred_element_type=F32)
    o = o / den.transpose(0, 1, 3, 4, 2)[..., None]
    lse = (m[..., 0] + jnp.log(den)).transpose(0, 1, 3, 4, 2)
    o = o.reshape(b, dilation, npad, h, hd)[:, :, :n].transpose(0, 2, 1, 3, 4).reshape(b, T, h, hd)
    lse = lse.reshape(b, dilation, npad, h)[:, :, :n].transpose(0, 2, 1, 3).reshape(b, T, h)
    return o, lse


def hybrid_attention(h, w_qkv, rpb, w_o):
    b, T, _ = h.shape
    qkv = (h @ w_qkv).reshape(b, T, 3, N_HEADS_QKV, HEAD_DIM)
    q, k, v = qkv[:, :, 0], qkv[:, :, 1], qkv[:, :, 2]
    o_a = neighbourhood_attention(q[:, :, :N_HEADS_A], k[:, :, :N_HEADS_A], v[:, :, :N_HEADS_A], rpb)
    pos = jnp.arange(T, dtype=F32)
    q_b = rope(q[:, :, N_HEADS_A:], pos)
    k_b = rope(k[:, :, N_HEADS_A:], pos)
    v_b = v[:, :, N_HEADS_A:]
    outs, lses = [], []
    for g, (window, dilation) in enumerate(B_PAIRS):
        sl = slice(g * N_HEADS_B_GROUP, (g + 1) * N_HEADS_B_GROUP)
        o_g, l_g = dilated_group(q_b[:, :, sl], k_b[:, :, sl], v_b[:, :, sl], window, dilation)
        outs.append(o_g)
        lses.append(l_g)
    wts = jax.nn.softmax(jnp.stack(lses, axis=0), axis=0)
    o_b = jnp.sum(wts[..., None] * jnp.stack(outs, axis=0), axis=0)
    o = jnp.concatenate([o_a.reshape(b, T, -1), o_b.astype(h.dtype).reshape(b, T, -1)], axis=-1)
    return o @ w_o


def swiglu(h, w_gate, w_up, w_down):
    return (jax.nn.silu(h @ w_gate) * (h @ w_up)) @ w_down


def depthwise_conv(x, w, bias):
    C = x.shape[-1]
    out = lax.conv_general_dilated(x, w[:, None, :], window_strides=(1,),
                                   padding=[(SSM_CONV // 2, SSM_CONV // 2)],
                                   dimension_numbers=('NWC', 'WIO', 'NWC'),
                                   feature_group_count=C)
    return out + bias


def ssd_chunked(xs, dt, A, Bm, Cm):
    b, T, G, HPG, P = xs.shape
    N = Bm.shape[-1]
    L = SSM_CHUNK
    nc = T // L
    dt = dt.reshape(b, T, G, HPG)
    a = dt * A.reshape(G, HPG)

    def chunks(t):
        return jnp.moveaxis(t.reshape((b, nc, L) + t.shape[2:]), 1, 0)

    tril = jnp.tril(jnp.ones((L, L), dtype=bool))

    def step(state, inp):
        x_c, dt_c, a_c, b_c, c_c = inp
        acum = jnp.cumsum(a_c, axis=1)
        seg = acum[:, :, None] - acum[:, None, :]
        decay = jnp.exp(jnp.where(tril[None, :, :, None, None], seg, -jnp.inf))
        cb = jnp.einsum('blgn,bsgn->blsg', c_c, b_c)
        w = cb[..., None] * decay * dt_c[:, None]
        y = jnp.einsum('blsgh,bsghp->blghp', w, x_c)
        y = y + jnp.einsum('blgn,bghpn->blghp', c_c, state) * jnp.exp(acum)[..., None]
        to_end = jnp.exp(acum[:, -1:] - acum) * dt_c
        state = state * jnp.exp(acum[:, -1])[..., None, None] + jnp.einsum('bsgn,bsgh,bsghp->bghpn', b_c, to_end, x_c)
        return state, y

    state0 = jnp.zeros((b, G, HPG, P, N), F32)
    _, ys = lax.scan(step, state0, (chunks(xs.astype(F32)), chunks(dt), chunks(a),
                                    chunks(Bm.astype(F32)), chunks(Cm.astype(F32))))
    return jnp.moveaxis(ys, 0, 1).reshape(b, T, G, HPG, P)


def mamba2_mixer(h, w_in, conv_w, conv_b, dt_bias, a_log, d_skip, g_gate, w_out):
    b, T, _ = h.shape
    zxbcdt = h @ w_in
    z = zxbcdt[..., :D_INNER]
    xbc = zxbcdt[..., D_INNER:D_INNER + CONV_DIM]
    dt_raw = zxbcdt[..., D_INNER + CONV_DIM:].reshape(b, T, 2, SSM_HEADS)
    xbc = jax.nn.silu(depthwise_conv(xbc, conv_w, conv_b))
    gn = SSM_GROUPS * SSM_STATE
    xs = xbc[..., :D_INNER].reshape(b, T, SSM_GROUPS, SSM_HPG, SSM_HEAD_DIM)
    Bm = xbc[..., D_INNER:D_INNER + gn].reshape(b, T, SSM_GROUPS, SSM_STATE)
    Cm = xbc[..., D_INNER + gn:].reshape(b, T, SSM_GROUPS, SSM_STATE)
    dt = jax.nn.softplus(dt_raw.astype(F32) + dt_bias.astype(F32))
    A = -jnp.exp(a_log.astype(F32))
    y_f = ssd_chunked(xs, dt[:, :, 0], A[0], Bm, Cm)
    y_b = ssd_chunked(xs[:, ::-1], dt[:, ::-1, 1], A[1], Bm[:, ::-1], Cm[:, ::-1])[:, ::-1]
    y = y_f + y_b + d_skip.astype(F32).reshape(SSM_GROUPS, SSM_HPG)[..., None] * xs.astype(F32)
    y = y.reshape(b, T, D_INNER) * jax.nn.silu(z.astype(F32))
    y = y * lax.rsqrt(jnp.mean(y * y, axis=-1, keepdims=True) + EPS) * g_gate.astype(F32)
    return y.astype(h.dtype) @ w_out


def moe_swiglu(h, w_router, w_gate, w_up, w_down):
    b, T, Dm = h.shape
    xf = h.reshape(-1, Dm)
    N = xf.shape[0]
    logits = jnp.einsum('nd,de->ne', xf, w_router, preferred_element_type=F32)
    top_val, top_idx = lax.top_k(logits, TOP_K)
    gates = jax.nn.softmax(top_val, axis=-1)
    e_flat = top_idx.reshape(-1)
    tok_flat = jnp.repeat(jnp.arange(N), TOP_K)
    g_flat = gates.reshape(-1)
    order = jnp.argsort(e_flat)
    es, ts, gs = e_flat[order], tok_flat[order], g_flat[order]
    counts = jnp.bincount(e_flat, length=N_EXPERTS)
    padded = ((counts + MOE_BLOCK - 1) // MOE_BLOCK) * MOE_BLOCK
    start = jnp.cumsum(counts) - counts
    ends = jnp.cumsum(padded)
    pstart = ends - padded
    dst = pstart[es] + (jnp.arange(N * TOP_K) - start[es])
    n_blocks = -(-(N * TOP_K) // MOE_BLOCK) + N_EXPERTS
    P = n_blocks * MOE_BLOCK
    tok_pad = jnp.full((P,), N, dtype=jnp.int32).at[dst].set(ts.astype(jnp.int32))
    gate_pad = jnp.zeros((P,), F32).at[dst].set(gs)
    blk_expert = jnp.minimum(jnp.searchsorted(ends, jnp.arange(n_blocks) * MOE_BLOCK, side='right'), N_EXPERTS - 1)
    x_pad = jnp.concatenate([xf, jnp.zeros((1, Dm), xf.dtype)], axis=0)[tok_pad].reshape(n_blocks, MOE_BLOCK, Dm)

    def expert_block(args):
        xb, e = args
        hid = jax.nn.silu(xb @ w_gate[e]) * (xb @ w_up[e])
        return hid @ w_down[e]

    y = lax.map(expert_block, (x_pad, blk_expert)).reshape(P, Dm).astype(F32) * gate_pad[:, None]
    out = jax.ops.segment_sum(y, tok_pad, num_segments=N + 1)[:N]
    return out.astype(h.dtype).reshape(b, T, Dm)


def _trunk(x, g_mix, g_ffn, w_qkv, rpb, w_o, w_ff_gate, w_ff_up, w_ff_down,
           w_in_c, conv_w, conv_b, dt_bias, a_log, d_skip, g_gate, w_out_c,
           w_router, w_e_gate, w_e_up, w_e_down, g_final):
    for layer in range(DEPTH):
        i = layer // 2
        if layer % 2 == 0:
            x = x + hybrid_attention(rmsnorm(x, g_mix[layer]), w_qkv[i], rpb[i], w_o[i])
            x = x + swiglu(rmsnorm(x, g_ffn[layer]), w_ff_gate[i], w_ff_up[i], w_ff_down[i])
        else:
            x = x + mamba2_mixer(rmsnorm(x, g_mix[layer]), w_in_c[i], conv_w[i], conv_b[i], dt_bias[i],
                                 a_log[i], d_skip[i], g_gate[i], w_out_c[i])
            x = x + moe_swiglu(rmsnorm(x, g_ffn[layer]), w_router[i], w_e_gate[i], w_e_up[i], w_e_down[i])
    return rmsnorm(x, g_final)


def setup_inputs(seed: int = 0) -> dict:
    key = jax.random.key(seed)
    ks = jax.random.split(key, 24)

    def nrm(k, shape, fan_in):
        return jax.random.normal(k, shape, F32) * (fan_in ** -0.5)

    def gain(k, shape):
        return 1.0 + 0.02 * jax.random.normal(k, shape, F32)

    dt0 = jnp.exp(jax.random.uniform(ks[13], (N_ODD, 2, SSM_HEADS), F32, math.log(1e-3), math.log(1e-1)))
    return {
        "x_prompt": jax.random.normal(ks[0], (BATCH, SEQ, D_MODEL), F32),
        "x_sample": jax.random.normal(ks[1], (DEC_BATCH, DEC_SEQ, D_MODEL), F32),
        "g_mix": gain(ks[2], (DEPTH, D_MODEL)),
        "g_ffn": gain(ks[3], (DEPTH, D_MODEL)),
        "w_qkv": nrm(ks[4], (N_EVEN, D_MODEL, 3 * N_HEADS_QKV * HEAD_DIM), D_MODEL),
        "rpb": 0.1 * jax.random.normal(ks[5], (N_EVEN, N_HEADS_A, 2 * WIN_H - 1, 2 * WIN_W - 1), F32),
        "w_o": nrm(ks[6], (N_EVEN, (N_HEADS_A + N_HEADS_B_GROUP) * HEAD_DIM, D_MODEL), (N_HEADS_A + N_HEADS_B_GROUP) * HEAD_DIM),
        "w_ff_gate": nrm(ks[7], (N_EVEN, D_MODEL, D_FF), D_MODEL),
        "w_ff_up": nrm(ks[8], (N_EVEN, D_MODEL, D_FF), D_MODEL),
        "w_ff_down": nrm(ks[9], (N_EVEN, D_FF, D_MODEL), D_FF),
        "w_in_c": nrm(ks[10], (N_ODD, D_MODEL, IN_PROJ_C), D_MODEL),
        "conv_w": nrm(ks[11], (N_ODD, SSM_CONV, CONV_DIM), SSM_CONV),
        "conv_b": 0.01 * jax.random.normal(ks[12], (N_ODD, CONV_DIM), F32),
        "dt_bias": dt0 + jnp.log(-jnp.expm1(-dt0)),
        "a_log": jnp.log(jax.random.uniform(ks[14], (N_ODD, 2, SSM_HEADS), F32, 1.0, 16.0)),
        "d_skip": 1.0 + 0.1 * jax.random.normal(ks[15], (N_ODD, SSM_HEADS), F32),
        "g_gate": gain(ks[16], (N_ODD, D_INNER)),
        "w_out_c": nrm(ks[17], (N_ODD, D_INNER, D_MODEL), D_INNER),
        "w_router": nrm(ks[18], (N_ODD, D_MODEL, N_EXPERTS), D_MODEL),
        "w_e_gate": nrm(ks[19], (N_ODD, N_EXPERTS, D_MODEL, D_FF_EXPERT), D_MODEL),
        "w_e_up": nrm(ks[20], (N_ODD, N_EXPERTS, D_MODEL, D_FF_EXPERT), D_MODEL),
        "w_e_down": nrm(ks[21], (N_ODD, N_EXPERTS, D_FF_EXPERT, D_MODEL), D_FF_EXPERT),
        "g_final": gain(ks[22], (D_MODEL,)),
    }


def reference(x_prompt, x_sample, g_mix, g_ffn, w_qkv, rpb, w_o, w_ff_gate, w_ff_up, w_ff_down,
              w_in_c, conv_w, conv_b, dt_bias, a_log, d_skip, g_gate, w_out_c,
              w_router, w_e_gate, w_e_up, w_e_down, g_final):
    y_prompt = _trunk(x_prompt, g_mix, g_ffn, w_qkv, rpb, w_o, w_ff_gate, w_ff_up, w_ff_down,
                      w_in_c, conv_w, conv_b, dt_bias, a_log, d_skip, g_gate, w_out_c,
                      w_router, w_e_gate, w_e_up, w_e_down, g_final)
    y_sample = _trunk(x_sample, g_mix, g_ffn, w_qkv, rpb, w_o, w_ff_gate, w_ff_up, w_ff_down,
                      w_in_c, conv_w, conv_b, dt_bias, a_log, d_skip, g_gate, w_out_c,
                      w_router, w_e_gate, w_e_up, w_e_down, g_final)
    return (y_prompt, y_sample)
```

```python
import math
import numpy as np
import ml_dtypes
import concourse.bass as bass
import concourse.mybir as mybir
from concourse.bass_utils import run_bass_kernel_spmd

F32 = mybir.dt.float32
BF16 = mybir.dt.bfloat16
AF = mybir.ActivationFunctionType
ALU = mybir.AluOpType
AX = mybir.AxisListType

T = 8192
D = 2048
NCORES = 8
NEG = -30000.0
ENGS = ["pe", "act", "dve", "pool", "sp"]
NDS = 24


class Op:
    __slots__ = ("eng", "fn", "deps", "signal", "sigval", "dma", "sem", "val", "n")


class Sched:
    def __init__(self, loop_n=None):
        self.loop_n = loop_n
        self.ops = {e: [] for e in ENGS}
        self.lastw = {}
        self.rd_c = {}
        self.rd_d = {}
        self.dmas = {e: [] for e in ENGS}
        self.pending_dma = []

    def add(self, eng, fn, r=(), w=(), dma=False):
        op = Op()
        op.eng, op.fn, op.dma, op.signal, op.sigval = eng, fn, dma, False, None
        deps = []
        for k in r:
            x = self.lastw.get(k)
            if x is not None:
                deps.append(x)
        for k in w:
            x = self.lastw.get(k)
            if x is not None:
                deps.append(x)
            deps.extend(self.rd_c.get(k, {}).values())
            deps.extend(self.rd_d.get(k, ()))
        for k in r:
            if dma:
                self.rd_d.setdefault(k, []).append(op)
            else:
                self.rd_c.setdefault(k, {})[eng] = op
        for k in w:
            self.lastw[k] = op
            self.rd_c[k] = {}
            self.rd_d[k] = []
        if dma:
            lst = self.dmas[eng]
            op.n = len(lst)
            if op.n >= NDS:
                deps.append(lst[op.n - NDS])
            lst.append(op)
            self.pending_dma.append(op)
        seen = set()
        od = []
        for d in deps:
            if d is op or id(d) in seen:
                continue
            if d.eng == "pe" and eng == "pe" and not d.dma and not dma:
                continue
            seen.add(id(d))
            od.append(d)
            if not d.dma:
                d.signal = True
        op.deps = od
        self.ops[eng].append(op)
        return op

    def barrier(self):
        lasts = [self.ops[e][-1] for e in ENGS if self.ops[e]]
        pend = list(self.pending_dma)
        self.pending_dma = []
        for e in ENGS:
            op = self.add(e, lambda eng: eng.nop())
            for d in lasts + pend:
                if d is op:
                    continue
                if not d.dma:
                    d.signal = True
                op.deps.append(d)
        self.lastw.clear()
        self.rd_c.clear()
        self.rd_d.clear()

    def emit(self, nc, engobj, sems, dsems, tot, ivar, tmpreg=None):
        per = {}
        for e in ENGS:
            c = 0
            for op in self.ops[e]:
                if op.dma:
                    op.sem = dsems[e][op.n % NDS]
                    op.val = 16 * (op.n // NDS + 1)
                    per[id(op.sem)] = max(per.get(id(op.sem), 0), op.val)
                elif op.signal:
                    c += 1
                    op.sigval = c
            per[id(sems[e])] = c

        def do_wait(e, eng, s, v):
            base = tot.get(id(s), 0)
            if ivar is None:
                eng.wait_ge(s, base + v)
                return
            t = tmpreg[e]
            eng.reg_mul(t, ivar, per[id(s)])
            eng.reg_add(t, t, base + v)
            eng.wait_ge(s, t)

        for e in ENGS:
            eng = engobj[e]
            waited = {}
            for op in self.ops[e]:
                for d in op.deps:
                    if d.dma:
                        s, v = d.sem, d.val
                    else:
                        s, v = sems[d.eng], d.sigval
                    key = id(s)
                    if waited.get(key, 0) >= v:
                        continue
                    waited[key] = v
                    do_wait(e, eng, s, v)
                ins = op.fn(eng)
                if op.dma:
                    ins.then_inc(op.sem, 16)
                elif op.signal:
                    ins.then_inc(sems[e], 1)
        n = 1 if self.loop_n is None else self.loop_n
        for k, v in per.items():
            tot[k] = tot.get(k, 0) + n * v


class Ctx:
    def __init__(self, nc):
        self.nc = nc
        self.S = Sched()
        self.segs = [self.S]
        self.cur_i = None
        self.arena_bytes = 212480
        self.arena = nc.alloc_sbuf_tensor("arena", [128, self.arena_bytes // 2], BF16)
        self.off = 0
        self.ps = [nc.alloc_psum_tensor(f"psb{i}", [128, 512], F32) for i in range(8)]

    def new_seg(self, loop_n=None):
        self.S.barrier()
        self.S = Sched(loop_n)
        self.segs.append(self.S)

    def reset(self):
        self.new_seg()
        self.off = 0

    def loop(self, n):
        self.new_seg(n)

    def end_loop(self):
        self.new_seg()

    def rs(self, a):
        return a(self.cur_i) if callable(a) else a

    def emit_all(self, sems, dsems):
        nc = self.nc
        self.S.barrier()
        engobj = {"pe": nc.tensor, "act": nc.scalar, "dve": nc.vector, "pool": nc.gpsimd, "sp": nc.sync}
        tot = {}
        tmpreg = {e: engobj[e].alloc_register(f"wt_{e}") for e in ENGS}
        for seg in self.segs:
            if seg.loop_n is None:
                self.cur_i = None
                seg.emit(nc, engobj, sems, dsems, tot, None)
            else:
                with nc.Fori(0, seg.loop_n) as i:
                    self.cur_i = i
                    seg.emit(nc, engobj, sems, dsems, tot, i, tmpreg)
                self.cur_i = None
        nc.all_engine_barrier()

    def sb(self, shape, dtype):
        n = int(np.prod(shape[1:]))
        esz = 4 if dtype == F32 else 2
        nb = (n * esz + 63) // 64 * 64
        assert self.off + nb <= self.arena_bytes, (self.off, nb)
        a = self.arena[0:shape[0], self.off // 2:(self.off + nb) // 2]
        self.off += nb
        if dtype != BF16:
            a = a.bitcast(dtype)
        a = a[:, 0:n]
        if len(shape) == 3:
            a = a.rearrange("p (a b) -> p a b", a=shape[1])
        elif len(shape) == 4:
            a = a.rearrange("p (a b c) -> p a b c", a=shape[1], b=shape[2])
        return a

    def psf(self, i):
        return self.ps[i][:]

    def psb(self, i):
        return self.ps[i][:].bitcast(BF16)

    def dma(self, q, out, in_, r=(), w=()):
        return self.S.add(q, lambda e: e.dma_start(out=self.rs(out), in_=self.rs(in_)), r, w, dma=True)

    def mm(self, out, lhsT, rhs, start, stop, r=(), w=()):
        return self.S.add("pe", lambda e: e.matmul(out, lhsT, rhs, start=start, stop=stop), r, w)

    def tr(self, out, in_, ident, r=(), w=()):
        return self.S.add("pe", lambda e: e.transpose(out, in_, ident), r, w)

    def act(self, out, in_, func, r=(), w=(), bias=None, scale=None, accum=None):
        kw = {}
        if bias is not None:
            kw["bias"] = bias
        if scale is not None:
            kw["scale"] = scale
        if accum is not None:
            kw["accum_out"] = accum
        return self.S.add("act", lambda e: e.activation(out, in_, func, **kw), r, w)

    def tt(self, eng, out, in0, in1, op, r=(), w=()):
        return self.S.add(eng, lambda e: e.tensor_tensor(out, in0, in1, op), r, w)

    def ts(self, eng, out, in0, s1, s2, op0, op1=None, r=(), w=(), accum=None):
        def f(e):
            kw = {}
            if op1 is not None:
                kw["op1"] = op1
            if accum is not None:
                kw["accum_out"] = accum
            return e.tensor_scalar(out, in0, s1, s2, op0, **kw)
        return self.S.add(eng, f, r, w)

    def stt(self, out, in0, scalar, in1, op0, op1, r=(), w=(), eng="dve"):
        return self.S.add(eng, lambda e: e.scalar_tensor_tensor(out, in0, scalar, in1, op0, op1), r, w)

    def cp(self, eng, out, in_, r=(), w=()):
        if eng == "act":
            return self.S.add("act", lambda e: e.copy(out, in_), r, w)
        return self.S.add(eng, lambda e: e.tensor_copy(out, in_), r, w)

    def recip(self, out, in_, r=(), w=()):
        return self.S.add("dve", lambda e: e.reciprocal(out, in_), r, w)

    def memset(self, eng, ap, val, r=(), w=()):
        return self.S.add(eng, lambda e: e.memset(ap, val), r, w)


def norm_tile(C, x_dram, t0, ntok, gt, xnT, tag, bufs, ident, ps_banks, kout, sb_src=None, keep32=None, sub_hook=None):
    xin, junk, xn, ss, epsb = bufs
    nsub = ntok // 128
    for s in range(nsub):
        b = s % 2
        if sb_src is None:
            kx = (tag, "xin", b)
            C.dma("sp", xin[b], x_dram(s), w=[kx])
            src = xin[b]
        else:
            src, kx = sb_src[s]
        kss = (tag, "ss", b)
        C.act(junk, src, AF.Square, r=[kx], w=[(tag, "junk"), kss], accum=ss[b][:, 0:1])
        C.act(ss[b][:, 1:2], ss[b][:, 0:1], AF.Sqrt, r=[kss, "epsb"], w=[(tag, "sq", b)], bias=epsb, scale=1.0 / D)
        C.recip(ss[b][:, 2:3], ss[b][:, 1:2], r=[(tag, "sq", b)], w=[(tag, "rs", b)])
        kxn = (tag, "xn", b)
        if keep32 is not None:
            x32, k32 = keep32[s]
            C.stt(x32, src, ss[b][:, 2:3], gt, ALU.mult, ALU.mult, r=[kx, (tag, "rs", b), (tag, "g")], w=[k32])
            C.cp("act", xn[b], x32, r=[k32], w=[kxn])
            if sub_hook is not None:
                sub_hook(s, x32, k32)
        else:
            C.stt(xn[b], src, ss[b][:, 2:3], gt, ALU.mult, ALU.mult, r=[kx, (tag, "rs", b), (tag, "g")], w=[kxn])
        for j in range(4):
            pb = ps_banks[(s * 4 + j) % len(ps_banks)]
            kp = ("ps", pb)
            pst = C.psb(pb)
            for i in range(4):
                kc = j * 4 + i
                C.tr(pst[:, i * 128:(i + 1) * 128], xn[b][:, kc * 128:(kc + 1) * 128], ident, r=[kxn, "ident"], w=[kp])
            srcp = pst[:, 0:512].rearrange("p (a b) -> p a b", a=4)
            dst = xnT[:, j * 4:(j + 1) * 4, s * 128:(s + 1) * 128]
            C.cp("act" if j % 2 == 0 else "dve", dst, srcp, r=[kp], w=[kout])


def norm_bufs(C, need_xin=True):
    xin = [C.sb([128, D], F32) for _ in range(2)] if need_xin else None
    junk = C.sb([128, D], BF16)
    xn = [C.sb([128, D], BF16) for _ in range(2)]
    ss = [C.sb([128, 4], F32) for _ in range(2)]
    epsb = C.sb([128, 1], F32)
    C.memset("dve", epsb, 1e-6, w=["epsb"])
    return (xin, junk, xn, ss, epsb)


def load_consts(C, io, gname, grow):
    ident = C.sb([128, 128], BF16)
    C.dma("sp", ident, io["ident_bf"], w=["ident"])
    gt = C.sb([128, D], F32)
    C.dma("sp", gt, io[gname][grow:grow + 1, :].partition_broadcast(128), w=[("nrm", "g")])
    return ident, gt


def stage_qkv(C, io, Ttot):
    C.reset()
    ident, gt = load_consts(C, io, "g_mix", 0)
    perm = C.sb([128, 128], BF16)
    C.dma("sp", perm, io["perm_bf"], w=["perm"])
    nb = norm_bufs(C, need_xin=False)
    xin4 = C.sb([128, 4, D], F32)
    xnT = [C.sb([128, 16, 512], BF16) for _ in range(2)]
    wbuf = [C.sb([128, 16, 512], BF16) for _ in range(2)]
    qks = C.sb([128, 32, 512], BF16)
    vs = C.sb([128, 4, D], BF16)
    cs = [C.sb([128, 512], F32) for _ in range(2)]
    sn = [C.sb([128, 512], F32) for _ in range(2)]
    qb = [C.sb([128, 512], BF16) for _ in range(2)]
    t1 = [C.sb([128, 512], F32) for _ in range(2)]
    t2 = [C.sb([128, 512], F32) for _ in range(2)]
    wv = io["w_qkv"].rearrange("(kc p) c -> p kc c", p=128)
    qk_d = io["qk_d"].rearrange("b p t -> p b t")
    wi = 0
    bi = 0
    ri = 0
    C.loop(Ttot // 512)
    for tt in range(1):
        xb = 0
        kx = ("xnT", xb)
        C.dma("sp", xin4, lambda i: io["x"][bass.ds(i * 512, 512), :].rearrange("(s p) c -> p s c", p=128), w=["xin4"])
        norm_tile(C, None, 0, 512, gt, xnT[xb], "nrm", nb, ident, [0, 1], kx, sb_src=[(xin4[:, s_, :], "xin4") for s_ in range(4)])
        C.dma("sp", cs[xb], lambda i: io["cos_t"][:, bass.ds(i * 512, 512)], w=[("cs", xb)])
        C.dma("sp", sn[xb], lambda i: io["sin_t"][:, bass.ds(i * 512, 512)], w=[("sn", xb)])
        for g in range(12):
            wb = wbuf[wi % 2]
            kw = ("w", wi % 2)
            wi += 1
            C.dma("pool", wb, wv[:, :, g * 512:(g + 1) * 512], w=[kw])
            if g < 8:
                for i in range(4):
                    b = g * 4 + i
                    bank = 2 + bi % 3
                    bi += 1
                    kp = ("ps", bank)
                    for kc in range(16):
                        C.mm(C.psf(bank), wb[:, kc, i * 128:(i + 1) * 128], xnT[xb][:, kc, :], kc == 0, kc == 15,
                             r=[kw, kx], w=[kp])
                    if b % 16 < 4:
                        C.cp("act", qks[:, b, :], C.psf(bank), r=[kp], w=[("qks", b)])
                    else:
                        rb = ri % 2
                        ri += 1
                        bank2 = 5 + rb
                        C.cp("act", qb[rb], C.psf(bank), r=[kp], w=[("qb", rb)])
                        C.mm(C.psf(bank2), perm, qb[rb], True, True, r=["perm", ("qb", rb)], w=[("ps", bank2)])
                        C.tt("dve", t1[rb], qb[rb], cs[xb], ALU.mult, r=[("qb", rb), ("cs", xb)], w=[("t1", rb)])
                        C.tt("dve", t2[rb], C.psf(bank2), sn[xb], ALU.mult, r=[("ps", bank2), ("sn", xb)], w=[("t2", rb)])
                        C.tt("dve", qks[:, b, :], t1[rb], t2[rb], ALU.add, r=[("t1", rb), ("t2", rb)], w=[("qks", b)])
            else:
                vg = g - 8
                for s in range(4):
                    bank = 2 + bi % 3
                    bi += 1
                    kp = ("ps", bank)
                    for kc in range(16):
                        C.mm(C.psf(bank), xnT[xb][:, kc, s * 128:(s + 1) * 128], wb[:, kc, :], kc == 0, kc == 15,
                             r=[kw, kx], w=[kp])
                    C.cp("act" if s % 2 else "dve", vs[:, s, vg * 512:(vg + 1) * 512], C.psf(bank), r=[kp], w=[("vs", s, vg)])
        C.dma("sp", lambda i: qk_d[:, :, bass.ds(i * 512, 512)], qks, r=[("qks", b) for b in range(32)], w=[("qk_d", tt)])
        C.dma("sp", lambda i: io["v_d"][bass.ds(i * 512, 512), :].rearrange("(s p) c -> p s c", p=128), vs,
              r=[("vs", s, vg) for s in range(4) for vg in range(4)], w=[("v_d", tt)])
    C.end_loop()


SCALE = 128 ** -0.5


class AttnBufs:
    pass


def attn_bufs(C):
    A = AttnBufs()
    A.ident = C.sb([128, 128], BF16)
    C.dma("sp", A.ident, C.io["ident_bf"], w=["ident"])
    A.P = [C.sb([128, 512], BF16) for _ in range(2)]
    A.PT = [C.sb([128, 4, 128], BF16) for _ in range(2)]
    A.cm = [C.sb([128, 16], F32) for _ in range(2)]
    A.sums = [C.sb([128, 16], F32) for _ in range(2)]
    A.st = [C.sb([128, 8], F32) for _ in range(2)]
    A.osb = [C.sb([128, 128], BF16) for _ in range(2)]
    A.n = 0
    A.sb_i = 0
    A.p_i = 0
    return A


def attn_tile(C, A, segs, rkeys, out_ap, wkey):
    t = A.n % 2
    A.n += 1
    nseg = len(segs)
    assert nseg <= 16
    cm, sums, st = A.cm[t], A.sums[t], A.st[t]
    kcm, ksum, kst = ("cm", t), ("sums", t), ("st", t)
    for si, (qT, kT, bias, vbl) in enumerate(segs):
        bank = A.sb_i % 3
        A.sb_i += 1
        n = kT.shape[-1]
        kp = ("ps", bank)
        C.mm(C.psf(bank)[:, 0:n], qT, kT, True, False, r=rkeys, w=[kp])
        C.mm(C.psf(bank)[:, 0:n], A.ident, bias, False, True, r=rkeys + ["ident"], w=[kp])
        C.S.add("dve", lambda e, o=cm[:, si:si + 1], i=C.psf(bank)[:, 0:n]: e.reduce_max(o, i, axis=AX.X), r=[kp], w=[kcm])
    C.S.add("dve", lambda e: e.reduce_max(st[:, 0:1], cm[:, 0:nseg], axis=AX.X), r=[kcm], w=[kst])
    C.ts("dve", st[:, 1:2], st[:, 0:1], -SCALE, None, ALU.mult, r=[kst], w=[kst])
    obank = 5 + t
    ko = ("ps", obank)
    nblk_total = sum(len(s[3]) for s in segs)
    bcount = 0
    for si, (qT, kT, bias, vbl) in enumerate(segs):
        bank = A.sb_i % 3
        A.sb_i += 1
        n = kT.shape[-1]
        kp = ("ps", bank)
        C.mm(C.psf(bank)[:, 0:n], qT, kT, True, False, r=rkeys, w=[kp])
        C.mm(C.psf(bank)[:, 0:n], A.ident, bias, False, True, r=rkeys + ["ident"], w=[kp])
        pi = A.p_i % 2
        A.p_i += 1
        P, PT = A.P[pi], A.PT[pi]
        kP, kPT = ("P", pi), ("PT", pi)
        C.act(P[:, 0:n], C.psf(bank)[:, 0:n], AF.Exp, r=[kp, kst], w=[kP, ksum], bias=st[:, 1:2], scale=SCALE,
              accum=sums[:, si:si + 1])
        tb = 3 + pi
        ktb = ("ps", tb)
        nb_ = n // 128
        for b in range(nb_):
            C.tr(C.psb(tb)[:, b * 128:(b + 1) * 128], P[:, b * 128:(b + 1) * 128], A.ident, r=[kP, "ident"], w=[ktb])
        src = C.psb(tb)[:, 0:nb_ * 128].rearrange("p (a b) -> p a b", a=nb_)
        C.cp("dve" if si % 2 else "act", PT[:, 0:nb_, :], src, r=[ktb], w=[kPT])
        for b in range(nb_):
            C.mm(C.psf(obank)[:, 0:128], PT[:, b, :], vbl[b], bcount == 0, bcount == nblk_total - 1,
                 r=[kPT] + rkeys, w=[ko])
            bcount += 1
    C.S.add("dve", lambda e: e.reduce_sum(st[:, 2:3], sums[:, 0:nseg], axis=AX.X), r=[ksum], w=[kst])
    C.recip(st[:, 3:4], st[:, 2:3], r=[kst], w=[kst])
    osb = A.osb[t]
    C.ts("dve", osb, C.psf(obank)[:, 0:128], st[:, 3:4], None, ALU.mult, r=[ko, kst], w=[("osb", t)])
    C.tr(C.psb(7)[:, t * 128:(t + 1) * 128], osb, A.ident, r=[("osb", t), "ident"], w=[("ps7", t)])
    C.cp("act", out_ap, C.psb(7)[:, t * 128:(t + 1) * 128], r=[("ps7", t)], w=[wkey])


def na_window(i, R):
    rs = [min(max(r - 4, 0), R - 8) for r in (2 * i, 2 * i + 1)]
    b0 = rs[0] // 2
    b1 = (rs[1] + 8 + 1) // 2
    nblk = b1 - b0
    pat = []
    for rl in range(2):
        r = 2 * i + rl
        row = []
        for rho in range(2 * nblk):
            kr = 2 * b0 + rho
            row.append(kr - r + 7 if rs[rl] <= kr < rs[rl] + 8 else None)
        pat.append(tuple(row))
    return b0, nblk, tuple(pat)


def build_TT(C, io):
    TT = C.sb([128, 4 * 15, 64], F32)
    rp = C.sb([128, 4 * 15 * 31], F32)
    Dj = C.sb([128, 31, 64], F32)
    ngm = C.sb([128, 64], F32)
    C.dma("sp", rp, io["rpb"].rearrange("a h r c -> a (h r c)").partition_broadcast(128), w=["rp"])
    C.dma("sp", Dj, io["na_dj"], w=["Dj"])
    C.dma("sp", ngm, io["na_neg"], w=["ngm"])
    C.ts("dve", rp, rp, 1.0 / SCALE, None, ALU.mult, r=["rp"], w=["rp"])
    for hd in range(60):
        C.cp("dve", TT[:, hd, :], ngm, r=["ngm"], w=[("TT", hd)])
        for j in range(31):
            C.stt(TT[:, hd, :], Dj[:, j, :], rp[:, hd * 31 + j:hd * 31 + j + 1], TT[:, hd, :], ALU.mult, ALU.add,
                  r=["Dj", "rp", ("TT", hd)], w=[("TT", hd)])
    return TT


def stage_attn(C, io, Ts):
    C.reset()
    C.io = io
    A = attn_bufs(C)
    nt = Ts // 128
    qk = io["qk_d"]
    vd = io["v_d"].rearrange("(b p) c -> p b c", p=128)
    oT_d = io["oT_d"]
    mark = C.off
    TT = build_TT(C, io)
    R = Ts // 64
    wins = [na_window(i, R) for i in range(nt)]
    variants = {}
    for (b0, nblk, pat) in wins:
        variants.setdefault((nblk, pat), len(variants))
    qT = C.sb([128, Ts], BF16)
    kT = C.sb([128, Ts], BF16)
    V = C.sb([128, nt, 128], BF16)
    oTs = C.sb([128, Ts], BF16)
    BT = {v: C.sb([128, 10 * 64], BF16) for v in variants.values()}
    for h in range(4):
        C.dma("sp", qT, qk[h], w=["qT"])
        C.dma("sp", kT, qk[16 + h], w=["kT"])
        C.dma("sp", V, vd[:, :, h * 128:(h + 1) * 128], w=["V"])
        for (nblk, pat), vi in variants.items():
            bt = BT[vi].rearrange("p (a b) -> p a b", b=64)
            for rl in range(2):
                for rho in range(2 * nblk):
                    dr = pat[rl][rho]
                    dst = bt[rl * 64:(rl + 1) * 64, rho, :]
                    if dr is None:
                        C.memset("dve", dst, NEG, w=[("BT", vi)])
                    else:
                        C.cp("dve", dst, TT[rl * 64:(rl + 1) * 64, h * 15 + dr, :], r=[("TT", h * 15 + dr)], w=[("BT", vi)])
        for i in range(nt):
            b0, nblk, pat = wins[i]
            vi = variants[(nblk, pat)]
            segs = []
            k0 = 0
            while k0 < nblk * 128:
                n = min(512, nblk * 128 - k0)
                segs.append((qT[:, i * 128:(i + 1) * 128], kT[:, b0 * 128 + k0:b0 * 128 + k0 + n], BT[vi][:, k0:k0 + n],
                             [V[:, b0 + (k0 // 128) + b, :] for b in range(n // 128)]))
                k0 += n
            attn_tile(C, A, segs, ["qT", "kT", "V", ("BT", vi)], oTs[:, i * 128:(i + 1) * 128], "oTs")
        C.dma("sp", oT_d[h], oTs, r=["oTs"], w=[("oT_d", h)])
    C.new_seg()
    C.off = mark
    GW = [(128, 384), (256, 640), (1024, 2176)]
    qg = [C.sb([128, Ts], BF16) for _ in range(3)]
    kg = [C.sb([128, Ts], BF16) for _ in range(3)]
    Vg = [C.sb([128, nt, 128], BF16) for _ in range(3)]
    oTs = C.sb([128, Ts], BF16)
    mk = [C.sb([128, GW[g][1]], BF16) for g in range(3)]
    for g in range(3):
        C.dma("sp", mk[g], io[f"dmask{g}"], w=[("mk", g)])
    for j in range(4):
        for g in range(3):
            hd = 4 + 4 * g + j
            C.dma("sp", qg[g], qk[hd], w=[("qg", g)])
            C.dma("sp", kg[g], qk[16 + hd], w=[("kg", g)])
            C.dma("sp", Vg[g], vd[:, :, hd * 128:(hd + 1) * 128], w=[("Vg", g)])
        rk = [("qg", g) for g in range(3)] + [("kg", g) for g in range(3)] + [("Vg", g) for g in range(3)] + \
             [("mk", g) for g in range(3)]
        for i in range(nt):
            t0 = i * 128
            segs = []
            for g in range(3):
                left, win = GW[g]
                lo = max(t0 - left, 0)
                hi = min(t0 - left + win, Ts)
                k0 = lo
                while k0 < hi:
                    n = min(512, hi - k0)
                    m0 = k0 - (t0 - left)
                    segs.append((qg[g][:, t0:t0 + 128], kg[g][:, k0:k0 + n], mk[g][:, m0:m0 + n],
                                 [Vg[g][:, k0 // 128 + b, :] for b in range(n // 128)]))
                    k0 += n
            attn_tile(C, A, segs, rk, oTs[:, t0:t0 + 128], "oTs")
        C.dma("sp", oT_d[4 + j], oTs, r=["oTs"], w=[("oT_d", 4 + j)])


def stage_ffn(C, io, Ttot):
    C.reset()
    ident, gt = load_consts(C, io, "g_ffn", 0)
    nb = norm_bufs(C)
    xt = C.sb([128, 4, D], F32)
    oTt = C.sb([128, 8, 512], BF16)
    xnT = C.sb([128, 16, 512], BF16)
    hT = C.sb([128, 44, 512], BF16)
    wb = [C.sb([128, 4096], BF16) for _ in range(4)]
    wd = [C.sb([128, 11, 512], BF16) for _ in range(2)]
    sg = [C.sb([128, 512], F32) for _ in range(2)]
    oT_d = io["oT_d"].rearrange("h p t -> p h t")
    wo = io["w_o"].rearrange("(kc p) c -> p kc c", p=128)
    wg = io["w_ff_gate"].rearrange("(kc p) c -> p kc c", p=128)
    wu = io["w_ff_up"].rearrange("(kc p) c -> p kc c", p=128)
    wdn = io["w_ff_down"].rearrange("(fc p) c -> p fc c", p=128)
    wi = 0
    di = 0
    bi = 0
    C.loop(Ttot // 512)
    for tt in range(1):
        C.dma("sp", oTt, lambda i: oT_d[:, :, bass.ds(i * 512, 512)], w=["oTt"])
        C.dma("sp", xt, lambda i: io["x"][bass.ds(i * 512, 512), :].rearrange("(s p) c -> p s c", p=128),
              w=[("xt", s) for s in range(4)])
        for cb in range(4):
            w = wb[wi % 4]
            kw = ("wb", wi % 4)
            wi += 1
            wv = w.rearrange("p (a b) -> p a b", a=8)
            C.dma("pool", wv, wo[:, :, cb * 512:(cb + 1) * 512], w=[kw])
            for s in range(4):
                bank = 1 + bi % 3
                bi += 1
                kp = ("ps", bank)
                for kc in range(8):
                    C.mm(C.psf(bank), oTt[:, kc, s * 128:(s + 1) * 128], wv[:, kc, :], kc == 0, kc == 7, r=["oTt", kw], w=[kp])
                C.tt("dve", xt[:, s, cb * 512:(cb + 1) * 512], C.psf(bank), xt[:, s, cb * 512:(cb + 1) * 512], ALU.add,
                     r=[kp, ("xt", s)], w=[("xt", s)])
        norm_tile(C, None, 0, 512, gt, xnT, "nrm", nb, ident, [0], "xnT", sb_src=[(xt[:, s, :], ("xt", s)) for s in range(4)])
        for f2 in range(22):
            w1 = wb[wi % 4]
            k1 = ("wb", wi % 4)
            wi += 1
            w2 = wb[wi % 4]
            k2 = ("wb", wi % 4)
            wi += 1
            w1v = w1.rearrange("p (a b) -> p a b", a=16)
            w2v = w2.rearrange("p (a b) -> p a b", a=16)
            C.dma("pool", w1v, wg[:, :, f2 * 256:(f2 + 1) * 256], w=[k1])
            C.dma("pool", w2v, wu[:, :, f2 * 256:(f2 + 1) * 256], w=[k2])
            for i in range(2):
                fb = f2 * 2 + i
                ba = 1 + bi % 3
                bi += 1
                bb = 1 + bi % 3
                bi += 1
                for kc in range(16):
                    C.mm(C.psf(ba), w1v[:, kc, i * 128:(i + 1) * 128], xnT[:, kc, :], kc == 0, kc == 15, r=[k1, "xnT"], w=[("ps", ba)])
                for kc in range(16):
                    C.mm(C.psf(bb), w2v[:, kc, i * 128:(i + 1) * 128], xnT[:, kc, :], kc == 0, kc == 15, r=[k2, "xnT"], w=[("ps", bb)])
                sgi = fb % 2
                C.act(sg[sgi], C.psf(ba), AF.Silu, r=[("ps", ba)], w=[("sg", sgi)])
                C.tt("dve", hT[:, fb, :], C.psf(bb), sg[sgi], ALU.mult, r=[("ps", bb), ("sg", sgi)], w=[("hT", fb)])
        for cb in range(4):
            for fp in range(4):
                w = wd[di % 2]
                kw = ("wd", di % 2)
                di += 1
                C.dma("pool", w, wdn[:, fp * 11:(fp + 1) * 11, cb * 512:(cb + 1) * 512], w=[kw])
                for s in range(4):
                    for k in range(11):
                        fb = fp * 11 + k
                        C.mm(C.psf(4 + s), hT[:, fb, s * 128:(s + 1) * 128], w[:, k, :], fp == 0 and k == 0, fp == 3 and k == 10,
                             r=[kw, ("hT", fb)], w=[("ps", 4 + s)])
            for s in range(4):
                C.tt("dve", xt[:, s, cb * 512:(cb + 1) * 512], C.psf(4 + s), xt[:, s, cb * 512:(cb + 1) * 512], ALU.add,
                     r=[("ps", 4 + s), ("xt", s)], w=[("xt", s)])
        C.dma("sp", lambda i: io["x2_d"][bass.ds(i * 512, 512), :].rearrange("(s p) c -> p s c", p=128), xt,
              r=[("xt", s) for s in range(4)], w=[("x2_d", tt)])
    C.end_loop()


def stage_inproj(C, io, Ttot):
    C.reset()
    ident, gt = load_consts(C, io, "g_mix", 1)
    nb = norm_bufs(C, need_xin=False)
    xin4 = C.sb([128, 4, D], F32)
    xnT = [C.sb([128, 16, 512], BF16) for _ in range(2)]
    wbuf = [C.sb([128, 16, 512], BF16) for _ in range(2)]
    zs = C.sb([128, 4, 4096], BF16)
    xs = C.sb([128, 48, 512], BF16)
    dts = C.sb([128, 4, 128], F32)
    dtb = C.sb([128, 128], F32)
    one = C.sb([128, 1], F32)
    tmp = [C.sb([128, 128], F32) for _ in range(2)]
    C.memset("dve", one, 1.0, w=["one"])
    C.dma("sp", dtb, io["dt_bias"].rearrange("a b c -> a (b c)").partition_broadcast(128), w=["dtb"])
    wv = io["w_in_c"].rearrange("(kc p) c -> p kc c", p=128)
    xb_d = io["xbcT_d"].rearrange("b p t -> p b t")
    wi = 0
    bi = 0
    C.loop(Ttot // 512)
    for tt in range(1):
        xb = 0
        kx = ("xnT", xb)
        C.dma("act", xin4, lambda i: io["x2_d"][bass.ds(i * 512, 512), :].rearrange("(s p) c -> p s c", p=128), w=["xin4"])
        norm_tile(C, None, 0, 512, gt, xnT[xb], "nrm", nb, ident, [0, 1], kx, sb_src=[(xin4[:, s_, :], "xin4") for s_ in range(4)])
        for cg in range(21):
            wb = wbuf[wi % 2]
            kw = ("w", wi % 2)
            wi += 1
            if cg < 20:
                C.dma("pool", wb, wv[:, :, cg * 512:(cg + 1) * 512], w=[kw])
            else:
                C.dma("pool", wb[:, :, 0:128], wv[:, :, 10240:10368], w=[kw])
            if cg < 8:
                for s in range(4):
                    bank = 2 + bi % 3
                    bi += 1
                    kp = ("ps", bank)
                    for kc in range(16):
                        C.mm(C.psf(bank), xnT[xb][:, kc, s * 128:(s + 1) * 128], wb[:, kc, :], kc == 0, kc == 15, r=[kw, kx], w=[kp])
                    C.cp("act" if s % 2 else "dve", zs[:, s, cg * 512:(cg + 1) * 512], C.psf(bank), r=[kp], w=[("zs", s, cg)])
            elif cg < 20:
                for i in range(4):
                    b = (cg - 8) * 4 + i
                    bank = 2 + bi % 3
                    bi += 1
                    kp = ("ps", bank)
                    for kc in range(16):
                        C.mm(C.psf(bank), wb[:, kc, i * 128:(i + 1) * 128], xnT[xb][:, kc, :], kc == 0, kc == 15, r=[kw, kx], w=[kp])
                    C.cp("act" if i % 2 else "dve", xs[:, b, :], C.psf(bank), r=[kp], w=[("xs", b)])
            else:
                for s in range(4):
                    bank = 2 + bi % 3
                    bi += 1
                    kp = ("ps", bank)
                    for kc in range(16):
                        C.mm(C.psf(bank)[:, 0:128], xnT[xb][:, kc, s * 128:(s + 1) * 128], wb[:, kc, 0:128], kc == 0, kc == 15,
                             r=[kw, kx], w=[kp])
                    tb = s % 2
                    C.tt("dve", tmp[tb], C.psf(bank)[:, 0:128], dtb, ALU.add, r=[kp, "dtb"], w=[("tmp", tb)])
                    C.act(tmp[tb], tmp[tb], AF.Exp, r=[("tmp", tb)], w=[("tmp", tb)])
                    C.act(dts[:, s, :], tmp[tb], AF.Ln, r=[("tmp", tb), "one"], w=[("dts", s)], bias=one, scale=1.0)
        C.dma("act", lambda i: io["z_d"][bass.ds(i * 512, 512), :].rearrange("(s p) c -> p s c", p=128), zs,
              r=[("zs", s, cg) for s in range(4) for cg in range(8)], w=[("z_d", tt)])
        C.dma("act", lambda i: xb_d[:, :, bass.ds(i * 512, 512)], xs, r=[("xs", b) for b in range(48)], w=[("xb_d", tt)])
        C.dma("act", lambda i: io["dt_d"][bass.ds(i * 512, 512), :].rearrange("(s p) c -> p s c", p=128), dts,
              r=[("dts", s) for s in range(4)], w=[("dt_d", tt)])
    C.end_loop()


def stage_conv(C, io, Ts):
    C.reset()
    ident = C.sb([128, 128], BF16)
    C.dma("sp", ident, io["ident_bf"], w=["ident"])
    cw = C.sb([128, 48, 5], F32)
    cb = C.sb([128, 48], F32)
    C.dma("sp", cw, io["conv_w_l"], w=["cw"])
    C.dma("sp", cb, io["conv_b_l"], w=["cb"])
    nt = Ts // 128
    xp = [C.sb([128, Ts + 4], BF16) for _ in range(2)]
    acc = [C.sb([128, Ts], F32) for _ in range(2)]
    xc = [C.sb([128, Ts], BF16) for _ in range(2)]
    stg = [C.sb([128, nt, 128], BF16) for _ in range(2)]
    for b in range(2):
        C.memset("dve", xp[b][:, 0:2], 0.0, w=[("xp", b)])
        C.memset("dve", xp[b][:, Ts + 2:Ts + 4], 0.0, w=[("xp", b)])
    xtok = io["xtok_d"].rearrange("(b p) c -> p b c", p=128)
    btok = io["btok_d"].rearrange("(b p) c -> p b c", p=128)
    bi = 0
    for cbk in range(48):
        b = cbk % 2
        C.dma("sp", xp[b][:, 2:Ts + 2], io["xbcT_d"][cbk], w=[("xp", b)])
        C.ts("dve", acc[b], xp[b][:, 0:Ts], cw[:, cbk, 0:1], None, ALU.mult, r=[("xp", b), "cw"], w=[("acc", b)])
        for k in range(1, 5):
            C.stt(acc[b], xp[b][:, k:k + Ts], cw[:, cbk, k:k + 1], acc[b], ALU.mult, ALU.add, r=[("xp", b), "cw", ("acc", b)], w=[("acc", b)])
        C.act(xc[b], acc[b], AF.Silu, r=[("acc", b), "cb"], w=[("xc", b)], bias=cb[:, cbk:cbk + 1], scale=1.0)
        if cbk < 40:
            for j0 in range(0, nt, 8):
                bank = bi % 4
                bi += 1
                kp = ("ps", bank)
                for j in range(8):
                    C.tr(C.psb(bank)[:, j * 128:(j + 1) * 128], xc[b][:, (j0 + j) * 128:(j0 + j + 1) * 128], ident, r=[("xc", b), "ident"], w=[kp])
                C.cp("act", stg[b][:, j0:j0 + 8, :], C.psb(bank).rearrange("p (a b) -> p a b", a=8), r=[kp], w=[("stg", b)])
            if cbk < 32:
                C.dma("sp", xtok[:, :, cbk * 128:(cbk + 1) * 128], stg[b], r=[("stg", b)], w=[("xtok", cbk)])
            else:
                C.dma("sp", btok[:, :, (cbk - 32) * 128:(cbk - 31) * 128], stg[b], r=[("stg", b)], w=[("btok", cbk)])
        if cbk >= 32:
            dst = io["BT_d"][cbk - 32] if cbk < 40 else io["CT_d"][cbk - 40]
            C.dma("sp", dst, xc[b], r=[("xc", b)], w=[("bc_d", cbk)])


def stage_ssd(C, io, Ts):
    C.reset()
    nchunk = Ts // 128
    ident = C.sb([128, 128], BF16)
    C.dma("sp", ident, io["ident_bf"], w=["ident"])
    onesf = C.sb([128, 128], F32)
    C.memset("dve", onesf, 1.0, w=["onesf"])
    epsb = C.sb([128, 1], F32)
    C.memset("dve", epsb, 1e-6, w=["epsb"])
    trif = C.sb([128, 128], F32)
    tri4 = C.sb([128, 4, 128], F32)
    tri4b = C.sb([128, 4, 128], BF16)
    negm4 = C.sb([128, 512], BF16)
    Ab = C.sb([128, 64], F32)
    Dsk = C.sb([128, 64], F32)
    gg = C.sb([128, 4096], F32)
    C.dma("sp", Dsk, io["d_skip"].partition_broadcast(128), w=["Dsk"])
    C.dma("sp", gg, io["g_gate"].partition_broadcast(128), w=["gg"])
    st32 = C.sb([128, 64, 64], F32)
    stb = C.sb([128, 4096], BF16)
    xt = [C.sb([128, 4096], BF16) for _ in range(2)]
    Bt = [C.sb([128, 1024], BF16) for _ in range(2)]
    BTc = [C.sb([128, 8, 128], BF16) for _ in range(2)]
    CTc = [C.sb([128, 8, 128], BF16) for _ in range(2)]
    dtc = [C.sb([128, 64], F32) for _ in range(2)]
    sm = [C.sb([128, 6, 64], F32) for _ in range(2)]
    xw = C.sb([128, 64, 64], BF16)
    cbm = C.sb([128, 8, 128], BF16)
    atri = [C.sb([128, 4, 128], F32) for _ in range(2)]
    eb4 = [C.sb([128, 4, 128], BF16) for _ in range(2)]
    ce4 = [C.sb([128, 4, 128], BF16) for _ in range(2)]
    dec = [C.sb([128, 128], BF16) for _ in range(4)]
    WT = [C.sb([128, 128], BF16) for _ in range(4)]
    ysb = C.sb([128, 4096], F32)
    yf = C.sb([128, 4096], F32)
    zc = C.sb([128, 4096], BF16)
    dx = C.sb([128, 64, 64], BF16)
    sz = C.sb([128, 4096], BF16)
    yn = C.sb([128, 4096], BF16)
    ynTs = C.sb([128, 32, 128], BF16)
    nss = C.sb([128, 4], F32)
    xtok = io["xtok_d"]
    btok = io["btok_d"]
    BT_d = io["BT_d"].rearrange("g p t -> p g t")
    CT_d = io["CT_d"].rearrange("g p t -> p g t")
    ynT_d = io["ynT_d"].rearrange("c p t -> p c t")
    hi = 0
    for dr in range(2):
        C.dma("sp", trif, io["tri_f"][dr], w=["trif"])
        C.dma("sp", tri4, io["tri4_f"][dr], w=["tri4"])
        C.dma("sp", negm4, io["negm4"][dr], w=["negm4"])
        C.cp("dve", tri4b, tri4, r=["tri4"], w=["tri4b"])
        C.dma("sp", Ab, io["a_log"][0, dr:dr + 1, :].partition_broadcast(128), w=["Ab"])
        C.act(Ab, Ab, AF.Exp, r=["Ab"], w=["Ab"])
        C.ts("dve", Ab, Ab, -1.0, None, ALU.mult, r=["Ab"], w=["Ab"])
        C.memset("dve", st32, 0.0, w=[("st32", g) for g in range(8)])
        C.memset("dve", stb, 0.0, w=[("stb", g) for g in range(8)])
        order = range(nchunk) if dr == 0 else range(nchunk - 1, -1, -1)
        for ci, c in enumerate(order):
            b = ci % 2
            r0, r1 = c * 128, (c + 1) * 128
            C.dma("sp", xt[b], xtok[r0:r1, :], w=[("xt", b)])
            C.dma("sp", Bt[b], btok[r0:r1, :], w=[("Bt", b)])
            C.dma("sp", BTc[b], BT_d[:, :, r0:r1], w=[("BTc", b)])
            C.dma("sp", CTc[b], CT_d[:, :, r0:r1], w=[("CTc", b)])
            C.dma("sp", dtc[b], io["dt_d"][r0:r1, dr * 64:(dr + 1) * 64], w=[("dtc", b)])
            if dr == 1:
                C.dma("sp", yf, io["yf_d"][r0:r1, :], w=["yf"])
                C.dma("sp", zc, io["z_d"][r0:r1, :], w=["zc"])
            a, acs, nacs, te, el = (sm[b][:, i, :] for i in range(5))
            ksm = ("sm", b)
            C.tt("dve", a, dtc[b], Ab, ALU.mult, r=[("dtc", b), "Ab"], w=[ksm])
            C.mm(C.psf(0)[:, 0:64], trif, a, True, True, r=["trif", ksm], w=[("ps", 0)])
            C.mm(C.psf(0)[:, 64:128], onesf, a, True, True, r=["onesf", ksm], w=[("ps", 0)])
            C.cp("act", acs, C.psf(0)[:, 0:64], r=[("ps", 0)], w=[ksm])
            C.ts("dve", nacs, C.psf(0)[:, 0:64], -1.0, None, ALU.mult, r=[("ps", 0)], w=[ksm])
            C.tt("dve", te, C.psf(0)[:, 64:128], acs, ALU.subtract, r=[("ps", 0), ksm], w=[ksm])
            C.act(te, te, AF.Exp, r=[ksm], w=[ksm])
            C.tt("dve", te, te, dtc[b], ALU.mult, r=[ksm, ("dtc", b)], w=[ksm])
            C.act(el, C.psf(0)[:, 64:128], AF.Exp, r=[("ps", 0)], w=[ksm])
            C.tt("dve", xw, xt[b].rearrange("p (h q) -> p h q", h=64), te.unsqueeze(2).to_broadcast([128, 64, 64]), ALU.mult,
                 r=[("xt", b), ksm], w=["xw"])
            for g4 in range(2):
                for gi in range(4):
                    g = g4 * 4 + gi
                    C.mm(C.psf(1)[:, gi * 128:(gi + 1) * 128], BTc[b][:, g, :], CTc[b][:, g, :], True, True,
                         r=[("BTc", b), ("CTc", b)], w=[("ps", 1)])
                C.tt("dve", cbm[:, g4 * 4:(g4 + 1) * 4, :], C.psf(1).rearrange("p (a b) -> p a b", a=4), tri4b, ALU.mult,
                     r=[("ps", 1), "tri4b"], w=[("cbm", g4)])
            for h4 in range(16):
                g = h4 // 2
                q = h4 % 2
                kat = ("atri", q)
                C.tt("dve", atri[q], tri4, a[:, h4 * 4:(h4 + 1) * 4].unsqueeze(2).to_broadcast([128, 4, 128]), ALU.mult,
                     r=["tri4", ksm], w=[kat])
                a2 = atri[q].rearrange("p a b -> p (a b)")
                C.mm(C.psf(2), onesf, a2, True, True, r=["onesf", kat], w=[("ps", 2)])
                C.mm(C.psf(3), onesf, a2, True, False, r=["onesf", kat], w=[("ps", 3)])
                C.mm(C.psf(3), ident, negm4, False, True, r=["ident", "negm4"], w=[("ps", 3)])
                C.act(eb4[q].rearrange("p a b -> p (a b)"), C.psf(2), AF.Exp, r=[("ps", 2)], w=[("eb4", q)])
                C.tt("dve", ce4[q], eb4[q], CTc[b][:, g:g + 1, :].to_broadcast([128, 4, 128]), ALU.mult,
                     r=[("eb4", q), ("CTc", b)], w=[("ce4", q)])
                for hh in range(4):
                    h = h4 * 4 + hh
                    d = hi % 4
                    hi += 1
                    C.act(dec[d], C.psf(3)[:, hh * 128:(hh + 1) * 128], AF.Exp, r=[("ps", 3), ksm], w=[("dec", d)],
                          bias=nacs[:, h:h + 1], scale=1.0)
                    C.stt(WT[d], dec[d], dtc[b][:, h:h + 1], cbm[:, g, :], ALU.mult, ALU.mult,
                          r=[("dec", d), ("dtc", b), ("cbm", g // 4)], w=[("WT", d)])
                    yb = 4 + g % 2
                    nbk = 6 + g % 2
                    col = (h % 8) * 64
                    C.mm(C.psf(yb)[:, col:col + 64], WT[d], xt[b][:, h * 64:(h + 1) * 64], True, False,
                         r=[("WT", d), ("xt", b)], w=[("ps", yb)])
                    C.mm(C.psf(yb)[:, col:col + 64], ce4[q][:, hh, :], stb[:, h * 64:(h + 1) * 64], False, True,
                         r=[("ce4", q), ("stb", g)], w=[("ps", yb)])
                    C.mm(C.psf(nbk)[:, col:col + 64], Bt[b][:, g * 128:(g + 1) * 128], xw[:, h, :], True, True,
                         r=[("Bt", b), "xw"], w=[("ps", nbk)])
                if q == 1:
                    yb = 4 + g % 2
                    nbk = 6 + g % 2
                    ys = ysb[:, g * 512:(g + 1) * 512]
                    if dr == 0:
                        C.cp("act", ys, C.psf(yb), r=[("ps", yb)], w=[("ysb", g)])
                    else:
                        C.tt("dve", ys, C.psf(yb), yf[:, g * 512:(g + 1) * 512], ALU.add, r=[("ps", yb), "yf"], w=[("ysb", g)])
                    sg3 = st32[:, g * 8:(g + 1) * 8, :]
                    C.tt("dve", sg3, sg3, el[:, g * 8:(g + 1) * 8].unsqueeze(2).to_broadcast([128, 8, 64]), ALU.mult,
                         r=[("st32", g), ksm], w=[("st32", g)])
                    C.tt("dve", sg3, sg3, C.psf(nbk).rearrange("p (a b) -> p a b", a=8), ALU.add, r=[("st32", g), ("ps", nbk)],
                         w=[("st32", g)])
                    C.cp("act", stb[:, g * 512:(g + 1) * 512], sg3.rearrange("p a b -> p (a b)"), r=[("st32", g)], w=[("stb", g)])
            ykeys = [("ysb", g) for g in range(8)]
            if dr == 0:
                C.dma("sp", io["yf_d"][r0:r1, :], ysb, r=ykeys, w=[("yf_d", c)])
            else:
                C.tt("dve", dx, xt[b].rearrange("p (h q) -> p h q", h=64), Dsk.unsqueeze(2).to_broadcast([128, 64, 64]), ALU.mult,
                     r=[("xt", b), "Dsk"], w=["dx"])
                C.tt("dve", ysb, ysb, dx.rearrange("p h q -> p (h q)"), ALU.add, r=ykeys + ["dx"], w=ykeys)
                C.act(sz, zc, AF.Silu, r=["zc"], w=["sz"])
                C.tt("dve", ysb, ysb, sz, ALU.mult, r=ykeys + ["sz"], w=ykeys)
                C.act(yn, ysb, AF.Square, r=ykeys, w=["yn", "nss"], accum=nss[:, 0:1])
                C.act(nss[:, 1:2], nss[:, 0:1], AF.Sqrt, r=["nss", "epsb"], w=["nss1"], bias=epsb, scale=1.0 / 4096)
                C.recip(nss[:, 2:3], nss[:, 1:2], r=["nss1"], w=["nss2"])
                C.stt(yn, ysb, nss[:, 2:3], gg, ALU.mult, ALU.mult, r=ykeys + ["nss2", "gg", "yn"], w=["yn"])
                for j0 in range(4):
                    for j in range(8):
                        kc = j0 * 8 + j
                        C.tr(C.psb(1)[:, j * 128:(j + 1) * 128], yn[:, kc * 128:(kc + 1) * 128], ident, r=["yn", "ident"], w=[("ps", 1)])
                    C.cp("act", ynTs[:, j0 * 8:(j0 + 1) * 8, :], C.psb(1).rearrange("p (a b) -> p a b", a=8), r=[("ps", 1)],
                         w=["ynTs"])
                C.dma("sp", ynT_d[:, :, r0:r1], ynTs, r=["ynTs"], w=[("ynT_d", c)])


        C.new_seg()


def stage_moe(C, io, Ttot):
    C.reset()
    ident, gt = load_consts(C, io, "g_ffn", 1)
    identf = C.sb([128, 128], F32)
    C.dma("sp", identf, io["ident_f"], w=["identf"])
    gfin = C.sb([128, D], F32)
    C.dma("sp", gfin, io["g_final"].partition_broadcast(128), w=["gfin"])
    wr = C.sb([128, 16, 8], F32)
    C.dma("sp", wr, io["w_router"].rearrange("(kc p) e -> p kc e", p=128), w=["wr"])
    nb = norm_bufs(C, need_xin=False)
    _, junk, xn, ss, epsb = nb
    acc = C.sb([128, 4, D], F32)
    hT = C.sb([128, 56, 512], BF16)
    xnT = C.sb([128, 16, 512], BF16)
    wb = [C.sb([128, 4096], BF16) for _ in range(4)]
    wd = [C.sb([128, 8, 512], BF16) for _ in range(2)]
    x32_ = C.sb([128, D], F32)
    x32 = [x32_, x32_]
    xT32 = C.sb([128, 16, 128], F32)
    sg = [C.sb([128, 512], BF16) for _ in range(2)]
    gates = C.sb([128, 4, 8], F32)
    lg = C.sb([128, 8], F32)
    mx8 = C.sb([128, 8], F32)
    rt = C.sb([128, 8], F32)
    ge = C.sb([128, 2, 8], F32)
    ynT_d = io["ynT_d"].rearrange("c p t -> p c t")
    wout = io["w_out_c"].rearrange("(kc p) c -> p kc c", p=128)
    weg = io["w_e_gate"]
    weu = io["w_e_up"]
    wed = io["w_e_down"]
    st = {"wi": 0, "di": 0, "bi": 0}

    def router(s, x32ap, k32):
        for j in range(4):
            bank = 1 + st["bi"] % 3
            st["bi"] += 1
            kp = ("ps", bank)
            for i in range(4):
                kc = j * 4 + i
                C.tr(C.psf(bank)[:, i * 128:(i + 1) * 128], x32ap[:, kc * 128:(kc + 1) * 128], identf, r=[k32, "identf"], w=[kp])
            C.cp("act", xT32[:, j * 4:(j + 1) * 4, :], C.psf(bank).rearrange("p (a b) -> p a b", a=4), r=[kp], w=["xT32"])
        bank = 1 + st["bi"] % 3
        st["bi"] += 1
        kp = ("ps", bank)
        for kc in range(16):
            C.mm(C.psf(bank)[:, 0:8], xT32[:, kc, :], wr[:, kc, :], kc == 0, kc == 15, r=["xT32", "wr"], w=[kp])
        C.cp("dve", lg, C.psf(bank)[:, 0:8], r=[kp], w=["lg"])
        C.S.add("dve", lambda e: e.max(mx8, lg), r=["lg"], w=["mx8"])
        C.tt("dve", rt[:, 0:1], mx8[:, 1:2], mx8[:, 0:1], ALU.subtract, r=["mx8"], w=["rt"])
        C.act(rt[:, 1:2], rt[:, 0:1], AF.Exp, r=["rt"], w=["rt"])
        C.ts("dve", rt[:, 2:3], rt[:, 1:2], 1.0, None, ALU.add, r=["rt"], w=["rt"])
        C.recip(rt[:, 3:4], rt[:, 2:3], r=["rt"], w=["rt"])
        C.tt("dve", rt[:, 4:5], rt[:, 1:2], rt[:, 3:4], ALU.mult, r=["rt"], w=["rt"])
        C.ts("dve", ge[:, 0, :], lg, mx8[:, 0:1], rt[:, 3:4], ALU.is_equal, ALU.mult, r=["lg", "mx8", "rt"], w=["ge"])
        C.ts("dve", ge[:, 1, :], lg, mx8[:, 1:2], rt[:, 4:5], ALU.is_equal, ALU.mult, r=["lg", "mx8", "rt"], w=["ge"])
        C.tt("dve", gates[:, s, :], ge[:, 0, :], ge[:, 1, :], ALU.add, r=["ge"], w=[("gates", s)])

    C.loop(Ttot // 512)
    for tt in range(1):
        C.dma("act", hT[:, 0:32, :], lambda i: ynT_d[:, :, bass.ds(i * 512, 512)], w=[("hT", fb) for fb in range(32)])
        C.dma("act", acc, lambda i: io["x2_d"][bass.ds(i * 512, 512), :].rearrange("(s p) c -> p s c", p=128),
              w=[("acc", s) for s in range(4)])
        for cb in range(4):
            for kp_ in range(4):
                w = wd[st["di"] % 2]
                kw = ("wd", st["di"] % 2)
                st["di"] += 1
                C.dma("pool", w, wout[:, kp_ * 8:(kp_ + 1) * 8, cb * 512:(cb + 1) * 512], w=[kw])
                for s in range(4):
                    for k in range(8):
                        kc = kp_ * 8 + k
                        C.mm(C.psf(4 + s), hT[:, kc, s * 128:(s + 1) * 128], w[:, k, :], kp_ == 0 and k == 0, kp_ == 3 and k == 7,
                             r=[kw, ("hT", kc)], w=[("ps", 4 + s)])
            for s in range(4):
                C.tt("dve", acc[:, s, cb * 512:(cb + 1) * 512], C.psf(4 + s), acc[:, s, cb * 512:(cb + 1) * 512], ALU.add,
                     r=[("ps", 4 + s), ("acc", s)], w=[("acc", s)])
        norm_tile(C, None, 0, 512, gt, xnT, "nrm", nb, ident, [0], "xnT", sb_src=[(acc[:, s, :], ("acc", s)) for s in range(4)],
                  keep32=[(x32[0], ("x32", 0)) for s in range(4)], sub_hook=router)
        for e in range(8):
            for f2 in range(28):
                w1 = wb[st["wi"] % 4]
                k1 = ("wb", st["wi"] % 4)
                st["wi"] += 1
                w2 = wb[st["wi"] % 4]
                k2 = ("wb", st["wi"] % 4)
                st["wi"] += 1
                w1v = w1.rearrange("p (a b) -> p a b", a=16)
                w2v = w2.rearrange("p (a b) -> p a b", a=16)
                C.dma("pool", w1, weg[e, f2], w=[k1])
                C.dma("pool", w2, weu[e, f2], w=[k2])
                for i in range(2):
                    fb = f2 * 2 + i
                    ba = 1 + st["bi"] % 3
                    st["bi"] += 1
                    bb = 1 + st["bi"] % 3
                    st["bi"] += 1
                    for kc in range(16):
                        C.mm(C.psf(ba), w1v[:, kc, i * 128:(i + 1) * 128], xnT[:, kc, :], kc == 0, kc == 15, r=[k1, "xnT"], w=[("ps", ba)])
                    for kc in range(16):
                        C.mm(C.psf(bb), w2v[:, kc, i * 128:(i + 1) * 128], xnT[:, kc, :], kc == 0, kc == 15, r=[k2, "xnT"], w=[("ps", bb)])
                    sgi = fb % 2
                    C.act(sg[sgi], C.psf(ba), AF.Silu, r=[("ps", ba)], w=[("sg", sgi)])
                    C.tt("dve", hT[:, fb, :], C.psf(bb), sg[sgi], ALU.mult, r=[("ps", bb), ("sg", sgi)], w=[("hT", fb)])
            for cb in range(4):
                for fp in range(8):
                    w = wd[st["di"] % 2]
                    kw = ("wd", st["di"] % 2)
                    st["di"] += 1
                    C.dma("pool", w[:, 0:7, :], wed[e, cb, fp].rearrange("p (k c) -> p k c", k=7), w=[kw])
                    for s in range(4):
                        for k in range(7):
                            fb = fp * 7 + k
                            C.mm(C.psf(4 + s), hT[:, fb, s * 128:(s + 1) * 128], w[:, k, :], fp == 0 and k == 0, fp == 7 and k == 6,
                                 r=[kw, ("hT", fb)], w=[("ps", 4 + s)])
                for s in range(4):
                    sl = acc[:, s, cb * 512:(cb + 1) * 512]
                    C.stt(sl, C.psf(4 + s), gates[:, s, e:e + 1], sl, ALU.mult, ALU.add,
                          r=[("ps", 4 + s), ("gates", s), ("acc", s)], w=[("acc", s)])
        for s in range(4):
            b = s % 2
            C.act(junk, acc[:, s, :], AF.Square, r=[("acc", s)], w=[("nrm", "junk"), ("fss", b)], accum=ss[b][:, 0:1])
            C.act(ss[b][:, 1:2], ss[b][:, 0:1], AF.Sqrt, r=[("fss", b), "epsb"], w=[("fsq", b)], bias=epsb, scale=1.0 / D)
            C.recip(ss[b][:, 2:3], ss[b][:, 1:2], r=[("fsq", b)], w=[("frs", b)])
            C.stt(acc[:, s, :], acc[:, s, :], ss[b][:, 2:3], gfin, ALU.mult, ALU.mult, r=[("acc", s), ("frs", b), "gfin"], w=[("acc", s)])
        C.dma("act", lambda i: io["y"][bass.ds(i * 512, 512), :].rearrange("(s p) c -> p s c", p=128), acc,
              r=[("acc", s) for s in range(4)], w=[("y", tt)])
    C.end_loop()


def host_consts(seqs=(T,)):
    c = {}
    c["ident_bf"] = np.eye(128, dtype=np.float32).astype(ml_dtypes.bfloat16)
    c["ident_f"] = np.eye(128, dtype=np.float32)
    pm = np.zeros((128, 128), np.float32)
    for j in range(128):
        pm[(j + 64) % 128, j] = 1.0
    c["perm_bf"] = pm.astype(ml_dtypes.bfloat16)
    half = 64
    inv = (10000.0 ** (-np.arange(half, dtype=np.float32) / half)).astype(np.float32)
    pos = np.concatenate([np.arange(t, dtype=np.float32) for t in seqs])
    ang = pos[None, :] * inv[:, None]
    cos = np.cos(ang).astype(np.float32)
    sin = np.sin(ang).astype(np.float32)
    c["cos_t"] = np.concatenate([cos, cos], 0)
    c["sin_t"] = np.concatenate([-sin, sin], 0)
    cq = np.arange(128)[:, None] % 64
    ck = np.arange(64)[None, :]
    cstart = np.clip(cq - 8, 0, 48)
    ok = (ck >= cstart) & (ck < cstart + 16)
    dj = np.zeros((128, 31, 64), np.float32)
    for j in range(31):
        dj[:, j, :] = ((ck - cq + 15) == j) & ok
    c["na_dj"] = dj
    c["na_neg"] = np.where(ok, 0.0, NEG).astype(np.float32)
    sI = np.arange(128)[:, None]
    lI = np.arange(128)[None, :]
    tri = np.stack([(sI <= lI), (sI >= lI)]).astype(np.float32)
    c["tri_f"] = tri
    c["tri4_f"] = np.repeat(tri[:, :, None, :], 4, axis=2).copy()
    c["negm4"] = np.tile(np.where(tri > 0, 0.0, NEG), (1, 1, 4)).astype(np.float32).astype(ml_dtypes.bfloat16)
    q = np.arange(128)[:, None]
    for g, (left, win, dil, rad) in enumerate([(128, 384, 1, 64), (256, 640, 4, 64), (1024, 2176, 16, 64)]):
        kk = np.arange(win)[None, :]
        rel = kk - left - q
        okm = (rel % dil == 0) & (np.abs(rel) <= rad * dil)
        c[f"dmask{g}"] = np.where(okm, 0.0, NEG).astype(np.float32).astype(ml_dtypes.bfloat16)
    return c


IN_SPECS = {
    "g_mix": ([2, D], F32), "g_ffn": ([2, D], F32), "w_qkv": ([D, 6144], F32), "rpb": ([1, 4, 15, 31], F32),
    "ident_bf": ([128, 128], BF16), "ident_f": ([128, 128], F32), "perm_bf": ([128, 128], BF16),
    "cos_t": ([128, T], F32), "sin_t": ([128, T], F32),
    "na_dj": ([128, 31, 64], F32), "na_neg": ([128, 64], F32),
    "w_o": ([1024, D], F32), "w_ff_gate": ([D, 5632], F32), "w_ff_up": ([D, 5632], F32), "w_ff_down": ([5632, D], F32),
    "w_in_c": ([D, 10368], F32), "dt_bias": ([1, 2, 64], F32), "a_log": ([1, 2, 64], F32), "d_skip": ([1, 64], F32),
    "g_gate": ([1, 4096], F32), "conv_w_l": ([128, 48, 5], F32), "conv_b_l": ([128, 48], F32),
    "tri_f": ([2, 128, 128], F32), "tri4_f": ([2, 128, 4, 128], F32), "negm4": ([2, 128, 512], BF16),
    "w_out_c": ([4096, D], F32), "w_router": ([D, 8], F32), "w_e_gate": ([8, 28, 128, 4096], F32), "w_e_up": ([8, 28, 128, 4096], F32),
    "w_e_down": ([8, 4, 8, 128, 3584], F32), "g_final": ([1, D], F32),
    "dmask0": ([128, 384], BF16), "dmask1": ([128, 640], BF16), "dmask2": ([128, 2176], BF16),
}


def scratch_specs(Tm):
    return {
        "qk_d": ([32, 128, Tm], BF16), "v_d": ([Tm, D], BF16), "oT_d": ([8, 128, Tm], BF16), "x2_d": ([Tm, D], F32),
        "z_d": ([Tm, 4096], BF16), "xbcT_d": ([48, 128, Tm], BF16), "dt_d": ([Tm, 128], F32),
        "xtok_d": ([Tm, 4096], BF16), "btok_d": ([Tm, 1024], BF16), "BT_d": ([8, 128, Tm], BF16), "CT_d": ([8, 128, Tm], BF16),
        "yf_d": ([Tm, 4096], F32), "ynT_d": ([32, 128, Tm], BF16),
    }


def build(seqs, stages, debug_outs=(), in_names=None, in_scratch=()):
    nc = bass.Bass("TRN2", target_bir_lowering=False)
    io = {}
    Ttot = sum(seqs)
    specs = dict(IN_SPECS)
    specs["x"] = ([Ttot, D], F32)
    specs["cos_t"] = ([128, Ttot], F32)
    specs["sin_t"] = ([128, Ttot], F32)
    for k, (shp, dt) in specs.items():
        if in_names is not None and k not in in_names:
            continue
        io[k] = nc.dram_tensor(k, shp, dt, kind="ExternalInput").ap()
    io["y"] = nc.dram_tensor("y", [Ttot, D], F32, kind="ExternalOutput").ap()
    sspec = scratch_specs(Ttot)
    for k, (shp, dt) in sspec.items():
        kind = "ExternalOutput" if k in debug_outs else ("ExternalInput" if k in in_scratch else "Internal")
        io[k] = nc.dram_tensor(k, shp, dt, kind=kind).ap()
    C = Ctx(nc)
    C.io = io
    sems = {e: nc.alloc_semaphore(f"s_{e}") for e in ENGS}
    dsems = {e: [nc.alloc_semaphore(f"d_{e}_{i}") for i in range(NDS)] for e in ("sp", "pool", "act")}

    def seq_io(off, Ts):
        d = dict(io)
        for k, (shp, dt) in sspec.items():
            d[k] = io[k][off:off + Ts, :] if shp[0] == Ttot else io[k][:, :, off:off + Ts]
        return d

    offs = []
    o = 0
    for Ts in seqs:
        offs.append((o, Ts))
        o += Ts
    if "qkv" in stages:
        stage_qkv(C, io, Ttot)
    if "attn" in stages:
        for (o, Ts) in offs:
            sio = seq_io(o, Ts)
            C.io = sio
            stage_attn(C, sio, Ts)
    C.io = io
    if "ffn" in stages:
        stage_ffn(C, io, Ttot)
    if "inproj" in stages:
        stage_inproj(C, io, Ttot)
    if "conv" in stages:
        for (o, Ts) in offs:
            stage_conv(C, seq_io(o, Ts), Ts)
    if "ssd" in stages:
        for (o, Ts) in offs:
            stage_ssd(C, seq_io(o, Ts), Ts)
    if "moe" in stages:
        stage_moe(C, io, Ttot)
    C.emit_all(sems, dsems)
    return nc


def prep_moe_weights(wg, wu, wd):
    g = np.ascontiguousarray(wg.reshape(8, 16, 128, 28, 256).transpose(0, 3, 2, 1, 4)).reshape(8, 28, 128, 4096)
    u = np.ascontiguousarray(wu.reshape(8, 16, 128, 28, 256).transpose(0, 3, 2, 1, 4)).reshape(8, 28, 128, 4096)
    d = np.ascontiguousarray(wd.reshape(8, 8, 7, 128, 4, 512).transpose(0, 4, 1, 3, 2, 5)).reshape(8, 4, 8, 128, 3584)
    return g, u, d


ALL_STAGES = ["qkv", "attn", "ffn", "inproj", "conv", "ssd", "moe"]
N_ACTIVE = 2


def kernel(x_prompt, x_sample, g_mix, g_ffn, w_qkv, rpb, w_o, w_ff_gate, w_ff_up, w_ff_down,
           w_in_c, conv_w, conv_b, dt_bias, a_log, d_skip, g_gate, w_out_c,
           w_router, w_e_gate, w_e_up, w_e_down, g_final):
    f = lambda a: np.ascontiguousarray(np.asarray(a, dtype=np.float32))
    x_prompt, x_sample = f(x_prompt), f(x_sample)
    seqs = [x_prompt.shape[1], x_sample.shape[1]]
    nc = build(seqs, ALL_STAGES)
    c = host_consts(seqs)
    weg_t, weu_t, wed_t = prep_moe_weights(f(w_e_gate)[0], f(w_e_up)[0], f(w_e_down)[0])
    cw = f(conv_w)[0]
    cbias = f(conv_b)[0]
    shared = {
        "g_mix": f(g_mix), "g_ffn": f(g_ffn), "w_qkv": f(w_qkv)[0], "rpb": f(rpb), "w_o": f(w_o)[0],
        "w_ff_gate": f(w_ff_gate)[0], "w_ff_up": f(w_ff_up)[0], "w_ff_down": f(w_ff_down)[0],
        "w_in_c": f(w_in_c)[0], "dt_bias": f(dt_bias), "a_log": f(a_log), "d_skip": f(d_skip), "g_gate": f(g_gate),
        "conv_w_l": np.ascontiguousarray(cw.reshape(5, 48, 128).transpose(2, 1, 0)),
        "conv_b_l": np.ascontiguousarray(cbias.reshape(48, 128).T),
        "w_out_c": f(w_out_c)[0], "w_router": f(w_router)[0], "w_e_gate": weg_t, "w_e_up": weu_t,
        "w_e_down": wed_t, "g_final": f(g_final).reshape(1, -1),
    }
    for k in IN_SPECS:
        if k in c:
            shared[k] = c[k]
    in_maps = []
    for i in range(N_ACTIVE):
        m = dict(shared)
        m["x"] = np.ascontiguousarray(np.concatenate([x_prompt[i], x_sample[i]], axis=0))
        in_maps.append(m)
    res = run_bass_kernel_spmd(nc, in_maps, core_ids=list(range(N_ACTIVE)))
    ys = [np.asarray(r["y"]) for r in res.results]
    y_prompt = np.stack([y[:seqs[0]] for y in ys], axis=0).astype(np.float32)
    y_sample = np.stack([y[seqs[0]:] for y in ys], axis=0).astype(np.float32)
    return (y_prompt, y_sample)
```
